# Optimizing a Trainium2 kernel written in Bass

```python
import math
import jax, jax.numpy as jnp
from jax import lax
import numpy as np

D_MODEL = 2048
BATCH = 8
SEQ = 2048
DEPTH = 1

D_MIX = D_MODEL
D_CONV = D_MIX // 2
D_ATTN = D_MIX - D_CONV
CONV_WIDTH = 31
ATTN_HEAD_DIM = 64
V_HEAD_DIM = 2 * ATTN_HEAD_DIM
N_ATTN_HEADS = D_ATTN // V_HEAD_DIM
Q_BLOCK = 128
D_IN_PROJ = 2 * D_CONV + 3 * D_ATTN
N_GROUPS = 8
EXPERTS_PER_GROUP = 8
N_EXPERTS = N_GROUPS * EXPERTS_PER_GROUP
TOP_K = 2
D_EXPERT = 512
MOE_BLOCK = 128
RMS_EPS = 1e-6
SUBLN_EPS = 1e-5
LN_EPS = 1e-5

kernel_name = "hybrid_conv_diffattn_hmoe"


def rmsnorm(x, w, eps=RMS_EPS):
    xf = x.astype(jnp.float32)
    y = xf * lax.rsqrt(jnp.mean(xf * xf, axis=-1, keepdims=True) + eps)
    return (y * w.astype(jnp.float32)).astype(x.dtype)


def conformer_conv(a, g, dw_w, dw_b, ln_w, ln_b):
    u = a * jax.nn.sigmoid(g)
    u = lax.conv_general_dilated(
        u, dw_w[:, None, :].astype(u.dtype), window_strides=(1,),
        padding=[(CONV_WIDTH - 1, 0)], dimension_numbers=("NWC", "WIO", "NWC"),
        feature_group_count=D_CONV) + dw_b
    uf = u.astype(jnp.float32)
    mu = jnp.mean(uf, axis=-1, keepdims=True)
    var = jnp.mean(jnp.square(uf - mu), axis=-1, keepdims=True)
    un = (uf - mu) * lax.rsqrt(var + LN_EPS) * ln_w.astype(jnp.float32) + ln_b.astype(jnp.float32)
    return jax.nn.silu(un).astype(a.dtype)


def diff_attention(q, k, v, lam_q1, lam_k1, lam_q2, lam_k2, subln_w, lambda_init):
    B, S = q.shape[0], q.shape[1]
    H, d = N_ATTN_HEADS, ATTN_HEAD_DIM
    q = q.reshape(B, S, H, 2, d)
    k = k.reshape(B, S, H, 2, d)
    v = v.reshape(B, S, H, V_HEAD_DIM)
    f32 = jnp.float32
    lam = (jnp.exp(jnp.sum(lam_q1.astype(f32) * lam_k1.astype(f32)))
           - jnp.exp(jnp.sum(lam_q2.astype(f32) * lam_k2.astype(f32))) + lambda_init)
    scale = d ** -0.5
    n_blk = S // Q_BLOCK
    qb = q.reshape(B, n_blk, Q_BLOCK, H, 2, d).transpose(1, 0, 2, 3, 4, 5)
    k_pos = jnp.arange(S)
    neg = jnp.finfo(f32).min

    def block(args):
        qi, i = args
        s = jnp.einsum("bqhcd,bkhcd->bhcqk", qi, k, preferred_element_type=f32) * scale
        q_pos = i * Q_BLOCK + jnp.arange(Q_BLOCK)
        causal = q_pos[:, None] >= k_pos[None, :]
        p = jax.nn.softmax(jnp.where(causal, s, neg), axis=-1)
        diff = p[:, :, 0] - lam * p[:, :, 1]
        return jnp.einsum("bhqk,bkhe->bqhe", diff.astype(v.dtype), v)

    o = lax.map(block, (qb, jnp.arange(n_blk)))
    o = o.transpose(1, 0, 2, 3, 4).reshape(B, S, H, V_HEAD_DIM)
    o = rmsnorm(o, subln_w, SUBLN_EPS) * (1.0 - lambda_init)
    return o.reshape(B, S, D_ATTN)


def expert_dispatch(u, expert_id, gate, w1, w3, w2):
    T, D = u.shape
    A = expert_id.shape[0]
    n_blocks = (A + N_EXPERTS * (MOE_BLOCK - 1) + MOE_BLOCK - 1) // MOE_BLOCK
    P = n_blocks * MOE_BLOCK
    token_id = jnp.arange(A, dtype=jnp.int32) // TOP_K
    order = jnp.argsort(expert_id)
    e_sorted = expert_id[order]
    counts = jnp.zeros((N_EXPERTS,), jnp.int32).at[expert_id].add(1)
    padded = (counts + MOE_BLOCK - 1) // MOE_BLOCK * MOE_BLOCK
    starts = jnp.cumsum(counts) - counts
    pad_ends = jnp.cumsum(padded)
    pad_starts = pad_ends - padded
    dest = pad_starts[e_sorted] + jnp.arange(A, dtype=jnp.int32) - starts[e_sorted]
    row_tok = jnp.zeros((P,), jnp.int32).at[dest].set(token_id[order])
    row_gate = jnp.zeros((P,), gate.dtype).at[dest].set(gate[order])
    block_expert = jnp.minimum(
        jnp.searchsorted(pad_ends, jnp.arange(n_blocks, dtype=jnp.int32) * MOE_BLOCK, side="right"),
        N_EXPERTS - 1)
    xb = u[row_tok].reshape(n_blocks, MOE_BLOCK, D)

    def run(args):
        xi, e = args
        hdn = jax.nn.silu(xi @ w1[e]) * (xi @ w3[e])
        return hdn @ w2[e]

    yb = lax.map(run, (xb, block_expert)).reshape(P, D)
    return jnp.zeros((T, D), u.dtype).at[row_tok].add(yb * row_gate[:, None].astype(u.dtype))


def hier_moe(u, w_group, b_group, w_expert_gate, b_expert_gate, w1, w3, w2):
    B, S, D = u.shape
    T = B * S
    ut = u.reshape(T, D)
    f32 = jnp.float32
    g_logits = (ut @ w_group).astype(f32) + b_group.astype(f32)
    g_prob = jax.nn.softmax(g_logits, axis=-1)
    _, g_sel = lax.top_k(g_logits, 1)
    g_w = jnp.take_along_axis(g_prob, g_sel, axis=-1)
    e_logits = ((ut @ w_expert_gate).astype(f32).reshape(T, N_GROUPS, EXPERTS_PER_GROUP)
                + b_expert_gate.astype(f32))
    e_in_group = e_logits[jnp.arange(T), g_sel[:, 0]]
    top_v, top_i = lax.top_k(e_in_group, TOP_K)
    gate = g_w * jax.nn.softmax(top_v, axis=-1)
    expert_id = (g_sel * EXPERTS_PER_GROUP + top_i).astype(jnp.int32)
    y = expert_dispatch(ut, expert_id.reshape(-1), gate.reshape(-1), w1, w3, w2)
    return y.reshape(B, S, D)


def setup_inputs(seed: int = 0) -> dict:
    key = jax.random.key(seed)
    ks = jax.random.split(key, 24)
    f32 = jnp.float32
    L = DEPTH

    def nrm(k, shape, scale):
        return jax.random.normal(k, shape, f32) * scale

    return {
        "x": nrm(ks[0], (BATCH, SEQ, D_MODEL), 1.0),
        "mix_norm_w": 1.0 + nrm(ks[1], (L, D_MODEL), 0.01),
        "w_in": nrm(ks[2], (L, D_MODEL, D_IN_PROJ), D_MODEL ** -0.5),
        "conv_dw_w": nrm(ks[3], (L, CONV_WIDTH, D_CONV), CONV_WIDTH ** -0.5),
        "conv_dw_b": nrm(ks[4], (L, D_CONV), 0.01),
        "conv_ln_w": 1.0 + nrm(ks[5], (L, D_CONV), 0.01),
        "conv_ln_b": nrm(ks[6], (L, D_CONV), 0.01),
        "lam_q1": nrm(ks[7], (L, ATTN_HEAD_DIM), 0.1),
        "lam_k1": nrm(ks[8], (L, ATTN_HEAD_DIM), 0.1),
        "lam_q2": nrm(ks[9], (L, ATTN_HEAD_DIM), 0.1),
        "lam_k2": nrm(ks[10], (L, ATTN_HEAD_DIM), 0.1),
        "attn_subln_w": 1.0 + nrm(ks[11], (L, V_HEAD_DIM), 0.01),
        "w_out": nrm(ks[12], (L, D_MIX, D_MODEL), D_MIX ** -0.5),
        "ffn_norm_w": 1.0 + nrm(ks[13], (L, D_MODEL), 0.01),
        "w_group": nrm(ks[14], (L, D_MODEL, N_GROUPS), D_MODEL ** -0.5),
        "b_group": nrm(ks[15], (L, N_GROUPS), 0.01),
        "w_expert_gate": nrm(ks[16], (L, D_MODEL, N_EXPERTS), D_MODEL ** -0.5),
        "b_expert_gate": nrm(ks[17], (L, N_GROUPS, EXPERTS_PER_GROUP), 0.01),
        "w1": nrm(ks[18], (L, N_EXPERTS, D_MODEL, D_EXPERT), D_MODEL ** -0.5),
        "w3": nrm(ks[19], (L, N_EXPERTS, D_MODEL, D_EXPERT), D_MODEL ** -0.5),
        "w2": nrm(ks[20], (L, N_EXPERTS, D_EXPERT, D_MODEL), D_EXPERT ** -0.5),
        "final_norm_w": 1.0 + nrm(ks[21], (D_MODEL,), 0.01),
    }


def reference(x, mix_norm_w, w_in, conv_dw_w, conv_dw_b, conv_ln_w, conv_ln_b,
              lam_q1, lam_k1, lam_q2, lam_k2, attn_subln_w, w_out, ffn_norm_w,
              w_group, b_group, w_expert_gate, b_expert_gate, w1, w3, w2, final_norm_w):
    h = x
    splits = [D_CONV, 2 * D_CONV, 2 * D_CONV + D_ATTN, 2 * D_CONV + 2 * D_ATTN]
    for l in range(DEPTH):
        lambda_init = 0.8 - 0.6 * math.exp(-0.3 * l)
        u = rmsnorm(h, mix_norm_w[l])
        proj = jnp.einsum("bsd,de->bse", u, w_in[l])
        conv_a, conv_g, q, k, v = jnp.split(proj, splits, axis=-1)
        y_conv = conformer_conv(conv_a, conv_g, conv_dw_w[l], conv_dw_b[l],
                                conv_ln_w[l], conv_ln_b[l])
        y_attn = diff_attention(q, k, v, lam_q1[l], lam_k1[l], lam_q2[l], lam_k2[l],
                                attn_subln_w[l], lambda_init)
        y_mix = jnp.concatenate([y_conv, y_attn], axis=-1)
        h = h + jnp.einsum("bse,ed->bsd", y_mix, w_out[l])
        h = h + hier_moe(rmsnorm(h, ffn_norm_w[l]), w_group[l], b_group[l],
                         w_expert_gate[l], b_expert_gate[l], w1[l], w3[l], w2[l])
    return rmsnorm(h, final_norm_w)
```

```python
import numpy as np
import concourse.bass as bass
import concourse.mybir as mybir
from concourse.bass_utils import run_bass_kernel_spmd

F32 = mybir.dt.float32
BF16 = mybir.dt.bfloat16
I32 = mybir.dt.int32
U32 = mybir.dt.uint32
ALU = mybir.AluOpType
AF = mybir.ActivationFunctionType
AX = mybir.AxisListType

CONV_W = 31
HALO = CONV_W - 1
RMS_EPS = 1e-6
LN_EPS = 1e-5
LAMBDA_INIT = 0.8 - 0.6 * 1.0
BIG = 1.0e30


class Cfg:
    def __init__(self, D=2048, S=2048, NG=8, DE=512, C=128, B=8, GW=None):
        self.D, self.S, self.NG, self.DE, self.C, self.B = D, S, NG, DE, C, B
        self.GW = GW or min(1024, D)
        self.KD = D // 128
        self.DC = D // 2
        self.NJ = self.DC // 128
        self.NH = self.DC // 128
        self.TB = 512
        self.NB = S // 512
        self.NT = S // 128
        self.NE = NG * 8
        self.KE = DE // 128
        self.NEC = 2 * self.NJ + 3 * self.NH
        self.OC = D // 256
        self.NR = NG + self.NE
        self.YB = min(512, D)
        self.NYB = D // self.YB
        self.TRASH = self.NE * C
        self.NSLOT = self.NE * C + 128
        o = 0
        self.o_cw = o; o += self.NJ * CONV_W
        self.o_cb = o; o += self.NJ
        self.o_lnw = o; o += self.NJ
        self.o_lnb = o; o += self.NJ
        self.o_lam = o; o += 256
        self.o_sub = o; o += 1
        self.o_rb = o; o += self.NR
        self.NPP = (o + 7) // 8 * 8


FULL = Cfg()


class _Op:
    __slots__ = ("eng", "fn", "deps", "signal", "val", "dma", "slot", "prev", "idx")

    def __init__(self, eng, fn, dma):
        self.eng, self.fn, self.dma = eng, fn, dma
        self.deps = []
        self.signal = dma
        self.val = None
        self.slot = None
        self.prev = None


class Prog:
    ENGS = ("pe", "act", "dve", "pool", "sp")
    NDMA = 8

    def __init__(self, nc):
        self.nc = nc
        self.ops = {e: [] for e in self.ENGS}
        self.last_w = {}
        self.rd = {}
        self.last_op = {e: None for e in self.ENGS}
        self.dma_ops = []

    def op(self, eng, fn, reads=(), writes=(), dma=False, same_ok=False):
        o = _Op(eng, fn, dma)
        deps = []
        for k in reads:
            w = self.last_w.get(k)
            if w is not None:
                deps.append(w)
        for k in writes:
            w = self.last_w.get(k)
            if w is not None:
                deps.append(w)
            r = self.rd.get(k)
            if r:
                deps.extend(r[0].values())
                deps.extend(r[1])
        seen = set()
        for d in deps:
            if d is o or id(d) in seen:
                continue
            seen.add(id(d))
            if (not d.dma) and d.eng == eng and eng == "pe":
                continue
            d.signal = True
            o.deps.append(d)
        for k in reads:
            r = self.rd.setdefault(k, ({}, []))
            if dma:
                r[1].append(o)
            else:
                r[0][eng] = o
        for k in writes:
            self.last_w[k] = o
            self.rd[k] = ({}, [])
        self.ops[eng].append(o)
        if dma:
            self.dma_ops.append(o)
        else:
            self.last_op[eng] = o
        return o

    def dma(self, eng, out, in_, reads=(), writes=(), **kw):
        return self.op(eng, lambda e: e.dma_start(out=out, in_=in_, **kw), reads, writes, dma=True)

    def barrier(self):
        deps = [o for o in self.last_op.values() if o is not None] + list(self.dma_ops)
        for d in deps:
            d.signal = True
        for e in self.ENGS:
            o = _Op(e, None, False)
            o.deps = list(deps)
            self.ops[e].append(o)
        self.dma_ops = []
        self.last_w = {}
        self.rd = {}

    def wait_all(self, eng):
        deps = [o for o in self.last_op.values() if o is not None] + list(self.dma_ops)
        for d in deps:
            d.signal = True
        o = _Op(eng, None, False)
        o.deps = list(deps)
        self.ops[eng].append(o)

    def emit(self, es):
        nc = self.nc
        csem = {e: es.enter_context(nc.semaphore("c_" + e)) for e in ("pe", "act", "dve", "pool")}
        dsem = {e: [es.enter_context(nc.semaphore("d_%s%d" % (e, i))) for i in range(self.NDMA)]
                for e in ("act", "pool", "sp")}
        for e in self.ENGS:
            cnt = 0
            dcnt = [0] * self.NDMA
            nd = 0
            lastd = [None] * self.NDMA
            for o in self.ops[e]:
                if o.fn is None:
                    continue
                if o.dma:
                    s = nd % self.NDMA
                    nd += 1
                    o.slot = s
                    o.prev = lastd[s]
                    dcnt[s] += 16
                    o.val = dcnt[s]
                    lastd[s] = o
                elif o.signal:
                    cnt += 1
                    o.val = cnt
        block = es.enter_context(nc.Block())
        self.stats = {}

        def run(ename, e):
            waited = {}
            nwait = 0
            for o in self.ops[ename]:
                need = {}
                deps = list(o.deps)
                if o.dma and o.prev is not None:
                    deps.append(o.prev)
                for d in deps:
                    sem = dsem[d.eng][d.slot] if d.dma else csem[d.eng]
                    k = id(sem)
                    if waited.get(k, 0) >= d.val:
                        continue
                    if k not in need or need[k][1] < d.val:
                        need[k] = (sem, d.val)
                for k, (sem, v) in need.items():
                    e.wait_ge(sem, v)
                    waited[k] = v
                    nwait += 1
                if o.fn is None:
                    continue
                ins = o.fn(e)
                if o.dma:
                    ins.then_inc(dsem[ename][o.slot], 16)
                elif o.signal:
                    ins.then_inc(csem[ename], 1)
            self.stats[ename] = (len(self.ops[ename]), nwait)

        block.tensor(lambda e: run("pe", e))
        block.scalar(lambda e: run("act", e))
        block.vector(lambda e: run("dve", e))
        block.gpsimd(lambda e: run("pool", e))
        block.sync(lambda e: run("sp", e))


class Ring:
    def __init__(self, nc, name, n, shape, dt):
        self.t = [nc.alloc_sbuf_tensor("r_%s%d" % (name, i), shape, dt) for i in range(n)]
        self.name = name
        self.i = -1

    def next(self):
        self.i += 1
        s = self.i % len(self.t)
        return self.t[s], (self.name, s)


def build(cfg, dbg=False, stop=9):
    from contextlib import ExitStack
    c = cfg
    D, S, KD, NJ, NH, NB, NT, NE, NG, DE, KE, C = c.D, c.S, c.KD, c.NJ, c.NH, c.NB, c.NT, c.NE, c.NG, c.DE, c.KE, c.C
    NR = c.NR
    nc = bass.Bass("TRN2", target_bir_lowering=False)
    P = Prog(nc)

    def dram(name, shape, dt, kind):
        return nc.dram_tensor(name, list(shape), dt, kind=kind)

    x_d = dram("x", [S, D], F32, "ExternalInput")
    win_d = dram("win", [c.NEC, 128, KD * 128], F32, "ExternalInput")
    wout_d = dram("wout", [c.OC, 128, KD * 256], F32, "ExternalInput")
    if stop >= 3:
        w1_d = dram("w1", [NE, 128, KD * DE], F32, "ExternalInput")
        w3_d = dram("w3", [NE, 128, KD * DE], F32, "ExternalInput")
        w2_d = dram("w2", [NE, 128, KE * D], F32, "ExternalInput")
    wr_d = dram("wr", [128, KD * NR], F32, "ExternalInput")
    mixw_d = dram("mixw", [128, D], F32, "ExternalInput")
    ffnw_d = dram("ffnw", [128, D], F32, "ExternalInput")
    finw_d = dram("finw", [128, D], F32, "ExternalInput")
    pp_d = dram("pp", [128, c.NPP], F32, "ExternalInput")
    out_d = dram("out", [S, D], F32, "ExternalOutput")
    h1s_d = dram("h1s", [S, D], F32, "ExternalOutput" if dbg else "Internal")
    xs_d = dram("xs", [c.NSLOT, D], BF16, "Internal")
    GW = c.GW
    NGA = D // GW
    ys2_d = dram("ys", [c.NSLOT * NGA, GW], F32, "Internal")
    ys_v = ys2_d.ap().rearrange("(n a) b -> n (a b)", a=NGA)
    if dbg:
        dbg_dest = dram("dbg_dest", [128, NT * 2], I32, "ExternalOutput")
        dbg_gate = dram("dbg_gate", [128, NT * 2], F32, "ExternalOutput")

    sb = lambda name, shape, dt: nc.alloc_sbuf_tensor("s_" + name, shape, dt)
    es = ExitStack()
    with es:
        ident_bf = sb("ident_bf", [128, 128], BF16)
        ident_f = sb("ident_f", [128, 128], F32)
        ones_f = sb("ones_f", [128, 128], F32)
        ones_bf = sb("ones_bf", [128, 128], BF16)
        maskT = sb("maskT", [128, 128], BF16)
        maskf = sb("maskf", [128, 128], F32)
        dif_i = sb("dif_i", [128, 128], I32)
        U_bf = sb("U_bf", [128, 128], BF16)
        iota_i = sb("iota_i", [128, NE], I32)
        iota_f = sb("iota_f", [128, NE], F32)
        pp = sb("pp", [128, c.NPP], F32)
        epsr = sb("epsr", [128, 1], F32)
        epsl = sb("epsl", [128, 1], F32)
        lamt = sb("lamt", [128, 8], F32)
        neglam = sb("neglam", [128, 1], F32)
        subs = sb("subs", [128, 1], F32)
        desti = sb("desti", [128, NT * 2], I32)
        gate = sb("gate", [128, NT * 2], F32)
        desty = sb("desty", [128, NT * 2 * NGA], I32)
        lamj = sb("lamj", [128, 64], F32)
        trp_i = sb("trp_i", [128, 1], I32)
        trp = sb("trp", [128, 2], F32)

        cw = pp[:, c.o_cw:c.o_cw + NJ * CONV_W]
        cbv = pp[:, c.o_cb:c.o_cb + NJ]
        lnw = pp[:, c.o_lnw:c.o_lnw + NJ]
        lnb = pp[:, c.o_lnb:c.o_lnb + NJ]
        lam = pp[:, c.o_lam:c.o_lam + 256]
        subw = pp[:, c.o_sub:c.o_sub + 1]
        rbias = pp[:, c.o_rb:c.o_rb + NR]

        P.dma("sp", pp[:], pp_d.ap(), writes=["pp"])
        regs = {}

        def _setup_regs(e):
            regs["xs"] = e.alloc_register("bc_xs")
            regs["ys"] = e.alloc_register("bc_ys")
            e.reg_mov(regs["xs"], c.NSLOT - 1)
            return e.reg_mov(regs["ys"], c.NSLOT * NGA - 1)
        P.op("pool", _setup_regs)
        P.op("pool", lambda e: e.memset(ident_f[:], 0.0), writes=["ident_f"])
        P.op("pool", lambda e: e.affine_select(out=ident_f[:], in_=ident_f[:], pattern=[[-1, 128]],
                                               compare_op=ALU.not_equal, fill=1.0, base=0, channel_multiplier=1),
             reads=["ident_f"], writes=["ident_f"])
        P.op("pool", lambda e: e.memset(ones_f[:], 1.0), writes=["ones_f"])
        P.op("pool", lambda e: e.iota(dif_i[:], [[-1, 128]], base=0, channel_multiplier=1), writes=["dif_i"])
        P.op("dve", lambda e: e.tensor_copy(maskf[:], dif_i[:]), reads=["dif_i"], writes=["maskf"])
        P.op("dve", lambda e: e.tensor_scalar(out=maskf[:], in0=maskf[:], scalar1=-1.0, scalar2=None, op0=ALU.mult),
             reads=["maskf"], writes=["maskf"])
        P.op("dve", lambda e: e.tensor_scalar(out=maskT[:], in0=maskf[:], scalar1=0.0, scalar2=None, op0=ALU.is_ge),
             reads=["maskf"], writes=["maskT"])
        P.op("dve", lambda e: e.tensor_scalar(out=U_bf[:], in0=maskf[:], scalar1=0.0, scalar2=None, op0=ALU.is_gt),
             reads=["maskf"], writes=["U_bf"])
        P.op("dve", lambda e: e.tensor_copy(ident_bf[:], ident_f[:]), reads=["ident_f"], writes=["ident_bf"])
        P.op("dve", lambda e: e.tensor_copy(ones_bf[:], ones_f[:]), reads=["ones_f"], writes=["ones_bf"])
        P.op("pool", lambda e: e.iota(iota_i[:], [[1, NE]], base=0, channel_multiplier=0), writes=["iota_i"])
        P.op("dve", lambda e: e.tensor_copy(iota_f[:], iota_i[:]), reads=["iota_i"], writes=["iota_f"])
        P.op("pool", lambda e: e.iota(trp_i[:], [[1, 1]], base=c.TRASH, channel_multiplier=1), writes=["trp_i"])
        P.op("dve", lambda e: e.tensor_copy(trp[:, 0:1], trp_i[:]), reads=["trp_i"], writes=["trp"])
        P.op("dve", lambda e: e.tensor_scalar(out=trp[:, 1:2], in0=trp[:, 0:1], scalar1=-1.0, scalar2=None, op0=ALU.mult),
             reads=["trp"], writes=["trp"])
        P.op("pool", lambda e: e.memset(epsr[:], RMS_EPS), writes=["epsr"])
        P.op("pool", lambda e: e.memset(epsl[:], LN_EPS), writes=["epsl"])
        P.op("pool", lambda e: e.memset(lamt[:], 0.0), writes=["lamt"])
        for t in range(2):
            P.op("dve", lambda e, t=t: e.tensor_tensor(out=lamj[:], in0=lam[:, 128 * t:128 * t + 64],
                                                       in1=lam[:, 128 * t + 64:128 * t + 128], op=ALU.mult),
                 reads=["pp", "lamt"], writes=["lamj"])
            P.op("dve", lambda e, t=t: e.reduce_sum(out=lamt[:, t:t + 1], in_=lamj[:], axis=AX.X),
                 reads=["lamj"], writes=["lamt"])
        P.op("act", lambda e: e.activation(out=lamt[:, 2:4], in_=lamt[:, 0:2], func=AF.Exp),
             reads=["lamt"], writes=["lamt"])
        P.op("dve", lambda e: e.tensor_tensor(out=lamt[:, 4:5], in0=lamt[:, 3:4], in1=lamt[:, 2:3], op=ALU.subtract),
             reads=["lamt"], writes=["lamt"])
        P.op("dve", lambda e: e.tensor_scalar(out=neglam[:], in0=lamt[:, 4:5], scalar1=-LAMBDA_INIT, scalar2=None,
                                              op0=ALU.add), reads=["lamt"], writes=["neglam"])
        P.op("dve", lambda e: e.tensor_scalar(out=subs[:], in0=subw, scalar1=1.0 - LAMBDA_INIT, scalar2=None,
                                              op0=ALU.mult), reads=["pp"], writes=["subs"])

        base_sb = (nc.sbuf_base, nc.sbuf_top)
        base_ps = (nc.psum_base, nc.psum_top)

        pacc = [nc.alloc_psum_tensor("pacc%d" % i, [128, 512], F32) for i in range(3)]
        ptr = nc.alloc_psum_tensor("ptr", [128, 8, 128], BF16)
        pst = [nc.alloc_psum_tensor("pst%d" % i, [128, 512], F32) for i in range(2)]
        po = [nc.alloc_psum_tensor("po%d" % i, [128, 512], F32) for i in range(2)]
        pacc_i = [-1]

        def next_pacc():
            pacc_i[0] += 1
            s = pacc_i[0] % 3
            return pacc[s], ("pacc", s)

        pst_i = [-1]

        def next_pst():
            pst_i[0] += 1
            s = pst_i[0] % 2
            return pst[s], ("pst", s)

        mixw = sb("mixw", [128, D], F32)
        xnT = sb("xnT", [128, KD, 512], BF16)
        ymixT = sb("ymixT", [128, KD, 512], BF16)
        kT = sb("kT", [128, NH, S], BF16)
        Vaug = sb("Vaug", [128, NT, NH, 130], BF16)
        vs = sb("vs", [128, NJ, 512], F32)
        halo = sb("halo", [128, NJ, HALO], F32)
        ucur = Ring(nc, "ucur", 1, [128, 512 + HALO], F32)
        xt = Ring(nc, "xt", 2, [128, D], F32)
        xnb = Ring(nc, "xnb", 2, [128, D], BF16)
        wb = Ring(nc, "wb", 3, [128, KD * 128], BF16)
        wob = Ring(nc, "wob", 2, [128, KD * 256], BF16)
        sg = Ring(nc, "sg", 1, [128, 512], F32)
        sq = Ring(nc, "sq", 1, [128, 512], F32)
        mean = sb("mean", [128, 512], F32)
        rln = sb("rln", [128, 512], F32)
        qT = Ring(nc, "qT", 2, [128, 512], BF16)
        vT = Ring(nc, "vT", 2, [128, 512], BF16)
        PT = Ring(nc, "PT", 3, [128, 512], BF16)
        o0 = sb("o0", [128, 4, 128], F32)
        diff = Ring(nc, "diff", 2, [128, 128], F32)
        dnb = Ring(nc, "dnb", 2, [128, 128], BF16)
        junk = sb("junk", [128, 128], F32)
        xp = Ring(nc, "xp", 2, [128, 256], F32)
        hp = Ring(nc, "hp", 2, [128, 256], F32)
        ss = sb("ss", [128, NT], F32)
        rs = sb("rs", [128, NT], F32)
        NA = NH * 8
        ast = sb("ast", [128, 4 * NA], F32)

        P.dma("sp", mixw[:], mixw_d.ap(), writes=["mixw"])
        P.op("pool", lambda e: e.memset(ss[:], 0.0), writes=["ss"])
        P.op("pool", lambda e: e.memset(ast[:], 0.0), writes=["ast"])
        P.op("pool", lambda e: e.memset(halo[:], 0.0), writes=[("halo", j) for j in range(NJ)])
        P.op("pool", lambda e: e.memset(Vaug[:, :, :, 128:130], 1.0), writes=["vones"])

        def load_w(ec):
            t, k = wb.next()
            P.dma("pool", t[:], win_d[ec], writes=[k])
            return t, k

        evac_flip = [0]

        class _Stop(Exception):
            pass
        try:
          for tb in range(NB):
            for i in range(4):
                ti = 4 * tb + i
                xt_, xk = xt.next()
                xn_, nk = xnb.next()
                P.dma("sp", xt_[:], x_d[ti * 128:(ti + 1) * 128, :], writes=[xk])
                P.op("act", lambda e, xt_=xt_, xn_=xn_, ti=ti: e.activation(
                    out=xn_[:], in_=xt_[:], func=AF.Square, accum_out=ss[:, ti:ti + 1]),
                    reads=[xk, "ss"], writes=[nk, ("ss", ti)])
                P.op("act", lambda e, ti=ti: e.activation(out=rs[:, ti:ti + 1], in_=ss[:, ti:ti + 1], func=AF.Sqrt,
                                                          scale=1.0 / D, bias=epsr[:, 0:1]),
                     reads=[("ss", ti), "epsr"], writes=[("rs", ti)])
                P.op("dve", lambda e, ti=ti: e.reciprocal(out=rs[:, ti:ti + 1], in_=rs[:, ti:ti + 1]),
                     reads=[("rs", ti)], writes=[("rs", ti)])
                P.op("dve", lambda e, xt_=xt_, xn_=xn_, ti=ti: e.scalar_tensor_tensor(
                    out=xn_[:], in0=xt_[:], scalar=rs[:, ti:ti + 1], in1=mixw[:], op0=ALU.mult, op1=ALU.mult),
                    reads=[xk, ("rs", ti), "mixw"], writes=[nk])
                R1 = min(8, KD)
                for r in range(KD // R1):
                    for kk in range(R1):
                        k = r * R1 + kk
                        P.op("pe", lambda e, kk=kk, k=k, xn_=xn_: e.transpose(
                            ptr[:, kk, :], xn_[:, k * 128:(k + 1) * 128], ident_bf[:]),
                            reads=[nk, "ident_bf"], writes=["ptr"])
                    eng = ("dve", "act")[evac_flip[0] % 2]
                    evac_flip[0] += 1
                    if eng == "dve":
                        fn = lambda e, r=r, i=i: e.tensor_copy(xnT[:, r * R1:(r + 1) * R1, i * 128:(i + 1) * 128], ptr[:, 0:R1, :])
                    else:
                        fn = lambda e, r=r, i=i: e.copy(xnT[:, r * R1:(r + 1) * R1, i * 128:(i + 1) * 128], ptr[:, 0:R1, :])
                    P.op(eng, fn, reads=["ptr"],
                         writes=[("xnT", r * R1 + kk, i) for kk in range(R1)])
            xnT_keys = lambda k: [("xnT", k, i) for i in range(4)]
            if stop == 0.1:
                raise _Stop()

            for j in range(NJ):
                wa, wak = load_w(j)
                pa, pak = next_pacc()
                for k in range(KD):
                    P.op("pe", lambda e, wa=wa, pa=pa, k=k: e.matmul(pa[:], wa[:, k * 128:(k + 1) * 128], xnT[:, k, :],
                                                                    start=(k == 0), stop=(k == KD - 1)),
                         reads=[wak] + xnT_keys(k), writes=[pak])
                wg, wgk = load_w(NJ + j)
                pg, pgk = next_pacc()
                for k in range(KD):
                    P.op("pe", lambda e, wg=wg, pg=pg, k=k: e.matmul(pg[:], wg[:, k * 128:(k + 1) * 128], xnT[:, k, :],
                                                                    start=(k == 0), stop=(k == KD - 1)),
                         reads=[wgk] + xnT_keys(k), writes=[pgk])
                sg_, sgk = sg.next()
                uc, uk = ucur.next()
                P.op("act", lambda e, sg_=sg_, pg=pg: e.activation(out=sg_[:], in_=pg[:], func=AF.Sigmoid),
                     reads=[pgk], writes=[sgk])
                P.op("dve", lambda e, uc=uc, pa=pa, sg_=sg_: e.tensor_tensor(out=uc[:, HALO:HALO + 512], in0=pa[:],
                                                                            in1=sg_[:], op=ALU.mult),
                     reads=[pak, sgk], writes=[uk])
                P.op("dve", lambda e, uc=uc, j=j: e.tensor_copy(uc[:, 0:HALO], halo[:, j, :]),
                     reads=[("halo", j)], writes=[uk + ("h",)])
                P.op("dve", lambda e, uc=uc, j=j: e.tensor_scalar(
                    out=vs[:, j, :], in0=uc[:, 0:512], scalar1=cw[:, j * CONV_W:j * CONV_W + 1],
                    scalar2=cbv[:, j:j + 1], op0=ALU.mult, op1=ALU.add),
                    reads=[uk, uk + ("h",), "pp"], writes=[("vs", j)])
                for t in range(1, CONV_W):
                    P.op("dve", lambda e, uc=uc, j=j, t=t: e.scalar_tensor_tensor(
                        out=vs[:, j, :], in0=uc[:, t:t + 512], scalar=cw[:, j * CONV_W + t:j * CONV_W + t + 1],
                        in1=vs[:, j, :], op0=ALU.mult, op1=ALU.add),
                        reads=[uk, uk + ("h",), ("vs", j)], writes=[("vs", j)], same_ok=True)
                P.op("dve", lambda e, uc=uc, j=j: e.tensor_copy(halo[:, j, :], uc[:, 512:512 + HALO]),
                     reads=[uk], writes=[("halo", j)])
                sq_, sqk = sq.next()
                P.op("act", lambda e, sq_=sq_, j=j: e.activation(out=sq_[:], in_=vs[:, j, :], func=AF.Square),
                     reads=[("vs", j)], writes=[sqk])
                P.op("pe", lambda e, j=j: e.matmul(pst[0][:], ones_f[:], vs[:, j, :], start=(j == 0), stop=(j == NJ - 1)),
                     reads=["ones_f", ("vs", j)], writes=[("pst", 0)])
                P.op("pe", lambda e, j=j, sq_=sq_: e.matmul(pst[1][:], ones_f[:], sq_[:], start=(j == 0), stop=(j == NJ - 1)),
                     reads=["ones_f", sqk], writes=[("pst", 1)])
            P.op("dve", lambda e: e.tensor_scalar(out=mean[:], in0=pst[0][:], scalar1=1.0 / c.DC, scalar2=None, op0=ALU.mult),
                 reads=[("pst", 0)], writes=["mean"])
            P.op("dve", lambda e: e.tensor_tensor(out=rln[:], in0=mean[:], in1=mean[:], op=ALU.mult),
                 reads=["mean"], writes=["rln"])
            P.op("dve", lambda e: e.scalar_tensor_tensor(out=rln[:], in0=pst[1][:], scalar=1.0 / c.DC, in1=rln[:],
                                                         op0=ALU.mult, op1=ALU.subtract),
                 reads=[("pst", 1), "rln"], writes=["rln"])
            P.op("act", lambda e: e.activation(out=rln[:], in_=rln[:], func=AF.Sqrt, bias=epsl[:, 0:1], scale=1.0),
                 reads=["rln", "epsl"], writes=["rln"])
            P.op("dve", lambda e: e.reciprocal(out=rln[:], in_=rln[:]), reads=["rln"], writes=["rln"])
            for j in range(NJ):
                P.op("dve", lambda e, j=j: e.tensor_tensor(out=vs[:, j, :], in0=vs[:, j, :], in1=mean[:], op=ALU.subtract),
                     reads=[("vs", j), "mean"], writes=[("vs", j)])
                P.op("dve", lambda e, j=j: e.tensor_tensor(out=vs[:, j, :], in0=vs[:, j, :], in1=rln[:], op=ALU.mult),
                     reads=[("vs", j), "rln"], writes=[("vs", j)])
                P.op("act", lambda e, j=j: e.activation(out=ymixT[:, j, :], in_=vs[:, j, :], func=AF.Silu,
                                                        scale=lnw[:, j:j + 1], bias=lnb[:, j:j + 1]),
                     reads=[("vs", j), "pp"], writes=[("ymixT", j)])

            if stop == 0.2:
                raise _Stop()
            import os as _os
            for h in range(int(_os.environ.get('HMAX', NH))):
                _sk = _os.environ.get('SKIP', '')
                qT_, qk = qT.next()
                if 'q' not in _sk:
                    wq, wqk = load_w(2 * NJ + h)
                    pq, pqk = next_pacc()
                    for k in range(KD):
                        P.op("pe", lambda e, wq=wq, pq=pq, k=k: e.matmul(pq[:], wq[:, k * 128:(k + 1) * 128], xnT[:, k, :],
                                                                        start=(k == 0), stop=(k == KD - 1)),
                             reads=[wqk] + xnT_keys(k), writes=[pqk])
                    P.op("act", lambda e, qT_=qT_, pq=pq: e.copy(qT_[:], pq[:]), reads=[pqk], writes=[qk])
                if 'k' not in _sk:
                    wk_, wkk = load_w(2 * NJ + NH + h)
                    pk, pkk = next_pacc()
                    for k in range(KD):
                        P.op("pe", lambda e, wk_=wk_, pk=pk, k=k: e.matmul(pk[:], wk_[:, k * 128:(k + 1) * 128], xnT[:, k, :],
                                                                          start=(k == 0), stop=(k == KD - 1)),
                             reads=[wkk] + xnT_keys(k), writes=[pkk])
                    P.op("dve", lambda e, pk=pk, h=h, tb=tb: e.tensor_copy(kT[:, h, tb * 512:(tb + 1) * 512], pk[:]),
                         reads=[pkk], writes=[("kT", h, tb)])
                if 'v' not in _sk:
                    wv, wvk = load_w(2 * NJ + 2 * NH + h)
                    pv, pvk = next_pacc()
                    for k in range(KD):
                        P.op("pe", lambda e, wv=wv, pv=pv, k=k: e.matmul(pv[:], wv[:, k * 128:(k + 1) * 128], xnT[:, k, :],
                                                                        start=(k == 0), stop=(k == KD - 1)),
                             reads=[wvk] + xnT_keys(k), writes=[pvk])
                    vT_, vTk = vT.next()
                    P.op("act", lambda e, vT_=vT_, pv=pv: e.copy(vT_[:], pv[:]), reads=[pvk], writes=[vTk])
                    for i in range(0 if 't' in _sk else 4):
                        P.op("pe", lambda e, vT_=vT_, i=i: e.transpose(ptr[:, 4 + i, :], vT_[:, i * 128:(i + 1) * 128], ident_bf[:]),
                             reads=[vTk, "ident_bf"], writes=["ptr"])
                    if 't' not in _sk:
                        if h % 2 == 0:
                            fn = lambda e, h=h, tb=tb: e.tensor_copy(Vaug[:, 4 * tb:4 * tb + 4, h, 0:128], ptr[:, 4:8, :])
                        else:
                            fn = lambda e, h=h, tb=tb: e.copy(Vaug[:, 4 * tb:4 * tb + 4, h, 0:128], ptr[:, 4:8, :])
                        P.op(("dve", "act")[h % 2], fn, reads=["ptr"], writes=[("V", 4 * tb + i, h) for i in range(4)])
                for cpt, half in (() if _os.environ.get('NOATT') else ((0, 0), (0, 1), (1, 0), (1, 1))):
                    plo = 64 * cpt
                    qa, qb = 2 * half, 2 * half + 1
                    for kc in range(4 * tb + qb + 1):
                        qlo = max(qa, kc - 4 * tb)
                        nq = qb - qlo + 1
                        ps_, psk = next_pst()
                        P.op("pe", lambda e, ps_=ps_, plo=plo, h=h, kc=kc, qT_=qT_, qlo=qlo, nq=nq, qb=qb: e.matmul(
                            ps_[:, 0:nq * 128], kT[plo:plo + 64, h, kc * 128:(kc + 1) * 128],
                            qT_[plo:plo + 64, qlo * 128:(qb + 1) * 128], start=True, stop=True),
                            reads=[("kT", h, kc // 4), qk], writes=[psk])
                        pt_, ptk = PT.next()
                        P.op("act", lambda e, pt_=pt_, ps_=ps_, nq=nq: e.activation(
                            out=pt_[:, 0:nq * 128], in_=ps_[:, 0:nq * 128], func=AF.Exp, scale=0.125),
                            reads=[psk], writes=[ptk])
                        if kc - 4 * tb >= qa:
                            P.op("dve", lambda e, pt_=pt_: e.tensor_tensor(out=pt_[:, 0:128], in0=pt_[:, 0:128],
                                                                            in1=maskT[:], op=ALU.mult),
                                 reads=[ptk, "maskT"], writes=[ptk])
                        for qi in range(qlo, qb + 1):
                            P.op("pe", lambda e, pt_=pt_, qi=qi, qlo=qlo, kc=kc, h=h, tb=tb: e.matmul(
                                po[qi % 2][:, 0:129], pt_[:, (qi - qlo) * 128:(qi - qlo + 1) * 128],
                                Vaug[:, kc, h, 0:129], start=(kc == 0), stop=(kc == 4 * tb + qi)),
                                reads=[ptk, ("V", kc, h), "vones"], writes=[("po", qi % 2)])
                    for qi in (qa, qb):
                        a0 = 4 * ((h * 2 + cpt) * 4 + qi)
                        pod = po[qi % 2]
                        P.op("dve", lambda e, pod=pod, qi=qi, a0=a0: e.reciprocal(out=ast[:, a0:a0 + 1],
                                                                                  in_=pod[:, 128:129]),
                             reads=[("po", qi % 2), "ast"], writes=[("ast", a0)])
                        if cpt == 0:
                            P.op("dve", lambda e, pod=pod, qi=qi, a0=a0: e.tensor_scalar(
                                out=o0[:, qi, :], in0=pod[:, 0:128], scalar1=ast[:, a0:a0 + 1], scalar2=None,
                                op0=ALU.mult), reads=[("po", qi % 2), ("ast", a0)], writes=[("o0", qi)])
                        else:
                            df, dfk = diff.next()
                            dn, dnk = dnb.next()
                            P.op("dve", lambda e, a0=a0: e.tensor_tensor(out=ast[:, a0 + 1:a0 + 2], in0=ast[:, a0:a0 + 1],
                                                                         in1=neglam[:], op=ALU.mult),
                                 reads=[("ast", a0), "neglam"], writes=[("ast", a0 + 1)])
                            P.op("dve", lambda e, pod=pod, qi=qi, a0=a0, df=df: e.scalar_tensor_tensor(
                                out=df[:], in0=pod[:, 0:128], scalar=ast[:, a0 + 1:a0 + 2], in1=o0[:, qi, :],
                                op0=ALU.mult, op1=ALU.add),
                                reads=[("po", qi % 2), ("ast", a0 + 1), ("o0", qi)], writes=[dfk])
                            P.op("act", lambda e, df=df, a0=a0: e.activation(out=junk[:], in_=df[:], func=AF.Square,
                                                                             accum_out=ast[:, a0 + 2:a0 + 3]),
                                 reads=[dfk, "ast"], writes=["junk", ("ast", a0 + 2)])
                            P.op("act", lambda e, a0=a0: e.activation(out=ast[:, a0 + 3:a0 + 4], in_=ast[:, a0 + 2:a0 + 3],
                                                                      func=AF.Sqrt, scale=1.0 / 128.0, bias=epsl[:, 0:1]),
                                 reads=[("ast", a0 + 2), "epsl"], writes=[("ast", a0 + 3)])
                            P.op("dve", lambda e, a0=a0: e.reciprocal(out=ast[:, a0 + 3:a0 + 4], in_=ast[:, a0 + 3:a0 + 4]),
                                 reads=[("ast", a0 + 3)], writes=[("ast", a0 + 3)])
                            P.op("dve", lambda e, df=df, dn=dn, a0=a0: e.tensor_scalar(
                                out=dn[:], in0=df[:], scalar1=ast[:, a0 + 3:a0 + 4], scalar2=None, op0=ALU.mult),
                                reads=[dfk, ("ast", a0 + 3)], writes=[dnk])
                            P.op("pe", lambda e, dn=dn, qi=qi: e.transpose(ptr[:, qi, :], dn[:], ident_bf[:]),
                                 reads=[dnk, "ident_bf"], writes=["ptr"])
                            P.op("act", lambda e, qi=qi, h=h: e.mul(ymixT[:, NJ + h, qi * 128:(qi + 1) * 128],
                                                                    ptr[:, qi, :], subs[:, 0:1]),
                                 reads=["ptr", "subs"], writes=[("ymixT", NJ + h, qi)])

            if stop == 0.3:
                raise _Stop()
            ymix_keys = lambda kk: [("ymixT", kk)] if kk < NJ else [("ymixT", kk, qi) for qi in range(4)]
            for oc in range(c.OC):
                wo, wok = wob.next()
                P.dma("pool", wo[:], wout_d[oc], writes=[wok])
                for i in range(4):
                    ti = 4 * tb + i
                    pa, pak = next_pacc()
                    for kk in range(KD):
                        P.op("pe", lambda e, pa=pa, kk=kk, i=i, wo=wo: e.matmul(
                            pa[:, 0:256], ymixT[:, kk, i * 128:(i + 1) * 128], wo[:, kk * 256:(kk + 1) * 256],
                            start=(kk == 0), stop=(kk == KD - 1)),
                            reads=[wok] + ymix_keys(kk), writes=[pak])
                    xp_, xpk = xp.next()
                    hp_, hpk = hp.next()
                    P.dma("sp", xp_[:], x_d[ti * 128:(ti + 1) * 128, oc * 256:(oc + 1) * 256], writes=[xpk])
                    P.op("dve", lambda e, hp_=hp_, pa=pa, xp_=xp_: e.tensor_tensor(out=hp_[:], in0=pa[:, 0:256], in1=xp_[:],
                                                                                    op=ALU.add),
                         reads=[pak, xpk], writes=[hpk])
                    P.dma("sp", h1s_d[ti * 128:(ti + 1) * 128, oc * 256:(oc + 1) * 256], hp_[:], reads=[hpk],
                          writes=[("h1s", ti, oc)])

        except _Stop:
            pass
        if stop < 2:
            P.wait_all("sp")
            P.emit(es)
            return nc, P
        P.barrier()
        nc.sbuf_base, nc.sbuf_top = base_sb
        nc.psum_base, nc.psum_top = base_ps
        ptf = nc.alloc_psum_tensor("ptf", [128, 4, 128], F32)
        plog = nc.alloc_psum_tensor("plog", [128, 512], F32)
        pcum = nc.alloc_psum_tensor("pcum", [128, 512], F32)
        ffnw = sb("ffnw", [128, D], F32)
        wr = sb("wr", [128, KD * NR], F32)
        ht = Ring(nc, "ht", 2, [128, D], F32)
        x2b = Ring(nc, "x2b", 2, [128, D], BF16)
        x2T = sb("x2T", [128, KD, 128], F32)
        Eb = Ring(nc, "Eb", 2, [128, NE], BF16)
        Eacc = sb("Eacc", [128, NE], BF16)
        zt = sb("zt", [128, D], F32)
        NS = 40
        rt = sb("rt", [128, NT * NS], F32)
        lg = Ring(nc, "lg", 2, [128, NR], F32)
        elm = Ring(nc, "elm", 2, [128, NE], F32)
        ohk = Ring(nc, "ohk", 2, [128, NE], F32)
        m8 = sb("m8", [128, NT * 8], F32)
        i8 = sb("i8", [128, NT * 8], U32)
        ss2 = sb("ss2", [128, NT], F32)
        rs2 = sb("rs2", [128, NT], F32)

        P.dma("sp", ffnw[:], ffnw_d.ap(), writes=["ffnw"])
        P.dma("sp", wr[:], wr_d.ap(), writes=["wr"])
        P.op("pool", lambda e: e.memset(zt[:], 0.0), writes=["zt"])
        P.op("pool", lambda e: e.memset(Eacc[:], 0.0), writes=["Eacc"])
        P.op("pool", lambda e: e.memset(ss2[:], 0.0), writes=["ss2"])
        P.op("pool", lambda e: e.memset(rt[:], 0.0), writes=["rt"])
        P.dma("sp", ys_v[c.TRASH:c.TRASH + 128, :], zt[:], reads=["zt"], writes=["ys_trash"])
        ztb = sb("ztb", [128, D], BF16)
        P.op("pool", lambda e: e.memset(ztb[:], 0.0), writes=["ztb"])
        for zi in range(c.NSLOT // 128):
            P.dma("sp", xs_d[zi * 128:(zi + 1) * 128, :], ztb[:], reads=["ztb"], writes=[("xsz", zi)])

        for ti in range(NT):
            ht_, hk = ht.next()
            xb_, xbk = x2b.next()
            P.dma("sp", ht_[:], h1s_d[ti * 128:(ti + 1) * 128, :], writes=[hk])
            P.op("act", lambda e, ht_=ht_, xb_=xb_, ti=ti: e.activation(out=xb_[:], in_=ht_[:], func=AF.Square,
                                                                        accum_out=ss2[:, ti:ti + 1]),
                 reads=[hk, "ss2"], writes=[xbk, ("ss2", ti)])
            P.op("act", lambda e, ti=ti: e.activation(out=rs2[:, ti:ti + 1], in_=ss2[:, ti:ti + 1], func=AF.Sqrt,
                                                      scale=1.0 / D, bias=epsr[:, 0:1]),
                 reads=[("ss2", ti), "epsr"], writes=[("rs2", ti)])
            P.op("dve", lambda e, ti=ti: e.reciprocal(out=rs2[:, ti:ti + 1], in_=rs2[:, ti:ti + 1]),
                 reads=[("rs2", ti)], writes=[("rs2", ti)])
            P.op("dve", lambda e, ht_=ht_, ti=ti: e.scalar_tensor_tensor(
                out=ht_[:], in0=ht_[:], scalar=rs2[:, ti:ti + 1], in1=ffnw[:], op0=ALU.mult, op1=ALU.mult),
                reads=[hk, ("rs2", ti), "ffnw"], writes=[hk])
            P.op("act", lambda e, ht_=ht_, xb_=xb_: e.copy(xb_[:], ht_[:]), reads=[hk], writes=[xbk])
            R2 = min(4, KD)
            for r in range(KD // R2):
                for kk in range(R2):
                    k = r * R2 + kk
                    P.op("pe", lambda e, kk=kk, k=k, ht_=ht_: e.transpose(ptf[:, kk, :], ht_[:, k * 128:(k + 1) * 128],
                                                                          ident_f[:]),
                         reads=[hk, "ident_f"], writes=["ptf"])
                eng = ("dve", "act")[r % 2]
                if eng == "dve":
                    fn = lambda e, r=r: e.tensor_copy(x2T[:, r * R2:(r + 1) * R2, :], ptf[:, 0:R2, :])
                else:
                    fn = lambda e, r=r: e.copy(x2T[:, r * R2:(r + 1) * R2, :], ptf[:, 0:R2, :])
                P.op(eng, fn, reads=["ptf"], writes=[("x2T", r)])
            for k in range(KD):
                P.op("pe", lambda e, k=k: e.matmul(plog[:, 0:NR], x2T[:, k, :], wr[:, k * NR:(k + 1) * NR],
                                                   start=(k == 0), stop=(k == KD - 1)),
                     reads=[("x2T", k // R2), "wr"], writes=["plog"])
            lg_, lgk = lg.next()
            el_, elk = elm.next()
            b0 = ti * NS
            col = lambda i: rt[:, b0 + i:b0 + i + 1]
            rk = lambda i: ("rt", ti, i)
            P.op("dve", lambda e, lg_=lg_: e.tensor_tensor(out=lg_[:], in0=plog[:, 0:NR], in1=rbias, op=ALU.add),
                 reads=["plog", "pp"], writes=[lgk])
            P.op("dve", lambda e, lg_=lg_, b0=b0: e.reduce_max(out=rt[:, b0:b0 + 1], in_=lg_[:, 0:NG], axis=AX.X),
                 reads=[lgk, "rt"], writes=[rk(0)])
            P.op("dve", lambda e, b0=b0: e.tensor_scalar(out=rt[:, b0 + 1:b0 + 2], in0=rt[:, b0:b0 + 1], scalar1=-1.0,
                                                         scalar2=None, op0=ALU.mult),
                 reads=[rk(0)], writes=[rk(1)])
            P.op("act", lambda e, lg_=lg_, b0=b0: e.activation(out=rt[:, b0 + 8:b0 + 8 + NG], in_=lg_[:, 0:NG], func=AF.Exp,
                                                               bias=rt[:, b0 + 1:b0 + 2], scale=1.0,
                                                               accum_out=rt[:, b0 + 2:b0 + 3]),
                 reads=[lgk, rk(1)], writes=[rk(2), rk(8)])
            P.op("dve", lambda e, b0=b0: e.reciprocal(out=rt[:, b0 + 3:b0 + 4], in_=rt[:, b0 + 2:b0 + 3]),
                 reads=[rk(2)], writes=[rk(3)])
            P.op("dve", lambda e, lg_=lg_, b0=b0: e.tensor_scalar(out=rt[:, b0 + 16:b0 + 16 + NG], in0=lg_[:, 0:NG],
                                                                  scalar1=rt[:, b0:b0 + 1], scalar2=None, op0=ALU.is_equal),
                 reads=[lgk, rk(0)], writes=[rk(16)])
            P.op("dve", lambda e, b0=b0: e.tensor_scalar(out=rt[:, b0 + 16:b0 + 16 + NG], in0=rt[:, b0 + 16:b0 + 16 + NG],
                                                         scalar1=-1.0, scalar2=BIG, op0=ALU.add, op1=ALU.mult),
                 reads=[rk(16)], writes=[rk(16)])
            P.op("dve", lambda e, lg_=lg_, el_=el_, b0=b0: e.tensor_tensor(
                out=el_[:].rearrange("p (g e) -> p g e", e=8),
                in0=lg_[:, NG:NR].rearrange("p (g e) -> p g e", e=8),
                in1=rt[:, b0 + 16:b0 + 16 + NG].unsqueeze(2).to_broadcast([128, NG, 8]), op=ALU.add),
                reads=[lgk, rk(16)], writes=[elk])
            P.op("dve", lambda e, el_=el_, ti=ti: e.max(out=m8[:, ti * 8:ti * 8 + 8], in_=el_[:]),
                 reads=[elk], writes=[("m8", ti)])
            P.op("dve", lambda e, el_=el_, ti=ti: e.max_index(out=i8[:, ti * 8:ti * 8 + 8], in_max=m8[:, ti * 8:ti * 8 + 8],
                                                              in_values=el_[:]),
                 reads=[elk, ("m8", ti)], writes=[("i8", ti)])
            P.op("dve", lambda e, ti=ti, b0=b0: e.tensor_tensor(out=rt[:, b0 + 4:b0 + 5], in0=m8[:, ti * 8 + 1:ti * 8 + 2],
                                                                in1=m8[:, ti * 8:ti * 8 + 1], op=ALU.subtract),
                 reads=[("m8", ti), "rt"], writes=[rk(4)])
            P.op("act", lambda e, b0=b0: e.activation(out=rt[:, b0 + 5:b0 + 6], in_=rt[:, b0 + 4:b0 + 5], func=AF.Exp),
                 reads=[rk(4)], writes=[rk(5)])
            P.op("dve", lambda e, b0=b0: e.tensor_scalar(out=rt[:, b0 + 6:b0 + 7], in0=rt[:, b0 + 5:b0 + 6], scalar1=1.0,
                                                         scalar2=None, op0=ALU.add), reads=[rk(5)], writes=[rk(6)])
            P.op("dve", lambda e, b0=b0: e.reciprocal(out=rt[:, b0 + 6:b0 + 7], in_=rt[:, b0 + 6:b0 + 7]),
                 reads=[rk(6)], writes=[rk(6)])
            P.op("dve", lambda e, b0=b0, ti=ti: e.tensor_tensor(out=gate[:, 2 * ti:2 * ti + 1], in0=rt[:, b0 + 6:b0 + 7],
                                                                in1=rt[:, b0 + 3:b0 + 4], op=ALU.mult),
                 reads=[rk(6), rk(3)], writes=[("gate", ti, 0)])
            P.op("dve", lambda e, b0=b0, ti=ti: e.tensor_tensor(out=gate[:, 2 * ti + 1:2 * ti + 2], in0=gate[:, 2 * ti:2 * ti + 1],
                                                                in1=rt[:, b0 + 5:b0 + 6], op=ALU.mult),
                 reads=[("gate", ti, 0), rk(5)], writes=[("gate", ti, 1)])
            Eb_, Ebk = Eb.next()
            P.op("dve", lambda e, Eb_=Eb_, el_=el_, ti=ti: e.tensor_scalar(out=Eb_[:], in0=el_[:],
                                                                           scalar1=m8[:, ti * 8 + 1:ti * 8 + 2], scalar2=None,
                                                                           op0=ALU.is_ge),
                 reads=[elk, ("m8", ti)], writes=[Ebk])
            P.op("pe", lambda e, Eb_=Eb_, ti=ti: e.matmul(pcum[:, 0:NE], U_bf[:], Eb_[:], start=True, stop=(ti == 0)),
                 reads=["U_bf", Ebk], writes=["pcum"])
            if ti > 0:
                P.op("pe", lambda e: e.matmul(pcum[:, 0:NE], ones_bf[:], Eacc[:], start=False, stop=True),
                     reads=["ones_bf", "Eacc"], writes=["pcum"])
            P.op("dve", lambda e, Eb_=Eb_: e.tensor_tensor(out=Eacc[:], in0=Eacc[:], in1=Eb_[:], op=ALU.add),
                 reads=["Eacc", Ebk], writes=["Eacc"])
            P.op("dve", lambda e, ti=ti, b0=b0: e.tensor_copy(rt[:, b0 + 24:b0 + 26], i8[:, ti * 8:ti * 8 + 2]),
                 reads=[("i8", ti), "rt"], writes=[rk(24)])
            for kx in range(2):
                oh_, ohkk = ohk.next()
                P.op("dve", lambda e, oh_=oh_, b0=b0, kx=kx: e.tensor_scalar(out=oh_[:], in0=iota_f[:],
                                                                             scalar1=rt[:, b0 + 24 + kx:b0 + 25 + kx],
                                                                             scalar2=None, op0=ALU.is_equal),
                     reads=["iota_f", rk(24)], writes=[ohkk])
                P.op("dve", lambda e, oh_=oh_: e.tensor_tensor(out=oh_[:], in0=oh_[:], in1=pcum[:, 0:NE], op=ALU.mult),
                     reads=[ohkk, "pcum"], writes=[ohkk])
                P.op("dve", lambda e, oh_=oh_, b0=b0, kx=kx: e.reduce_sum(out=rt[:, b0 + 26 + kx:b0 + 27 + kx], in_=oh_[:],
                                                                          axis=AX.X),
                     reads=[ohkk, "rt"], writes=[rk(26 + kx)])
            P.op("dve", lambda e, b0=b0: e.scalar_tensor_tensor(out=rt[:, b0 + 28:b0 + 30], in0=rt[:, b0 + 24:b0 + 26],
                                                                scalar=float(C), in1=rt[:, b0 + 26:b0 + 28],
                                                                op0=ALU.mult, op1=ALU.add),
                 reads=[rk(24), rk(26), rk(27)], writes=[rk(28)])
            P.op("dve", lambda e, b0=b0: e.tensor_scalar(out=rt[:, b0 + 30:b0 + 32], in0=rt[:, b0 + 26:b0 + 28],
                                                         scalar1=float(C), scalar2=None, op0=ALU.is_ge),
                 reads=[rk(26), rk(27)], writes=[rk(30)])
            P.op("dve", lambda e, b0=b0: e.tensor_scalar(out=rt[:, b0 + 30:b0 + 32], in0=rt[:, b0 + 30:b0 + 32],
                                                         scalar1=-1.0, scalar2=1.0, op0=ALU.mult, op1=ALU.add),
                 reads=[rk(30)], writes=[rk(30)])
            P.op("dve", lambda e, b0=b0: e.tensor_scalar(out=rt[:, b0 + 28:b0 + 30], in0=rt[:, b0 + 28:b0 + 30],
                                                         scalar1=trp[:, 1:2], scalar2=None, op0=ALU.add),
                 reads=[rk(28), "trp"], writes=[rk(28)])
            P.op("dve", lambda e, b0=b0: e.tensor_tensor(out=rt[:, b0 + 28:b0 + 30], in0=rt[:, b0 + 28:b0 + 30],
                                                         in1=rt[:, b0 + 30:b0 + 32], op=ALU.mult),
                 reads=[rk(28), rk(30)], writes=[rk(28)])
            P.op("dve", lambda e, b0=b0: e.tensor_scalar(out=rt[:, b0 + 28:b0 + 30], in0=rt[:, b0 + 28:b0 + 30],
                                                         scalar1=trp[:, 0:1], scalar2=None, op0=ALU.add),
                 reads=[rk(28), "trp"], writes=[rk(28)])
            P.op("dve", lambda e, b0=b0, ti=ti: e.tensor_tensor(out=gate[:, 2 * ti:2 * ti + 2], in0=gate[:, 2 * ti:2 * ti + 2],
                                                                in1=rt[:, b0 + 30:b0 + 32], op=ALU.mult),
                 reads=[("gate", ti, 0), ("gate", ti, 1), rk(30)], writes=[("gate", ti, 0), ("gate", ti, 1)])
            P.op("dve", lambda e, b0=b0, ti=ti: e.tensor_copy(desti[:, 2 * ti:2 * ti + 2], rt[:, b0 + 28:b0 + 30]),
                 reads=[rk(28)], writes=[("desti", ti)])
            for ga in range(NGA):
                P.op("dve", lambda e, b0=b0, ga=ga: e.tensor_scalar(out=rt[:, b0 + 32 + 2 * ga:b0 + 34 + 2 * ga],
                                                                    in0=rt[:, b0 + 28:b0 + 30], scalar1=float(NGA),
                                                                    scalar2=float(ga), op0=ALU.mult, op1=ALU.add),
                     reads=[rk(28), "rt"], writes=[rk(32 + 2 * ga)])
                P.op("dve", lambda e, b0=b0, ga=ga, ti=ti: e.tensor_copy(
                    desty[:, (ti * NGA + ga) * 2:(ti * NGA + ga) * 2 + 2], rt[:, b0 + 32 + 2 * ga:b0 + 34 + 2 * ga]),
                    reads=[rk(32 + 2 * ga)], writes=[("desty", ti, ga)])
            for kx in range(2):
                P.op("pool", lambda e, xb_=xb_, ti=ti, kx=kx: e.indirect_dma_start(
                    out=xs_d[:, :], out_offset=bass.IndirectOffsetOnAxis(ap=desti[:, 2 * ti + kx:2 * ti + kx + 1], axis=0),
                    in_=xb_[:, :], in_offset=None, bounds_check=regs["xs"], oob_is_err=False),
                    reads=[xbk, ("desti", ti)] + [("xsz", zi) for zi in range(c.NSLOT // 128)],
                    writes=[("xs", ti, kx)], dma=True)

        if dbg:
            P.dma("sp", dbg_dest.ap(), desti[:], reads=[("desti", ti) for ti in range(NT)], writes=["dbg_dest"])
            P.dma("sp", dbg_gate.ap(), gate[:], reads=[("gate", ti, k) for ti in range(NT) for k in range(2)],
                  writes=["dbg_gate"])

        if stop < 3:
            P.wait_all("sp")
            P.emit(es)
            return nc, P
        P.barrier()
        mark_sb = (nc.sbuf_base, nc.sbuf_top)
        nc.psum_base, nc.psum_top = base_ps
        nc.sbuf_base, nc.sbuf_top = base_sb
        ph1 = nc.alloc_psum_tensor("ph1", [128, 512], F32)
        ph3 = nc.alloc_psum_tensor("ph3", [128, 512], F32)
        ptr2 = nc.alloc_psum_tensor("ptr2", [128, 8, 128], BF16)
        py = [nc.alloc_psum_tensor("py%d" % i, [128, 512], F32) for i in range(4)]
        w1b = Ring(nc, "w1b", 2, [128, KD * DE], BF16)
        w3b = Ring(nc, "w3b", 2, [128, KD * DE], BF16)
        w2b = Ring(nc, "w2b", 2, [128, KE * D], BF16)
        xg = Ring(nc, "xg", 2, [128, D], BF16)
        xgT = Ring(nc, "xgT", 2, [128, KD, 128], BF16)
        s1 = Ring(nc, "s1", 2, [128, DE], F32)
        hdn = Ring(nc, "hdn", 2, [128, DE], BF16)
        hdT = Ring(nc, "hdT", 2, [128, KE, 128], BF16)
        ysb = Ring(nc, "ysb", 2, [128, D], F32)
        NCB = C // 128
        py_i = [-1]

        for ex in range(NE):
            w1_, w1k = w1b.next()
            w3_, w3k = w3b.next()
            w2_, w2k = w2b.next()
            P.dma("pool", w1_[:], w1_d[ex], writes=[w1k])
            P.dma("pool", w3_[:], w3_d[ex], writes=[w3k])
            P.dma("pool", w2_[:], w2_d[ex], writes=[w2k])
            for cbk in range(NCB):
                r0 = ex * C + cbk * 128
                xg_, xgk = xg.next()
                xgT_, xgTk = xgT.next()
                P.dma("sp", xg_[:], xs_d[r0:r0 + 128, :], writes=[xgk])
                R3 = min(8, KD)
                for r in range(KD // R3):
                    for kk in range(R3):
                        k = r * R3 + kk
                        P.op("pe", lambda e, kk=kk, k=k, xg_=xg_: e.transpose(ptr2[:, kk, :], xg_[:, k * 128:(k + 1) * 128],
                                                                              ident_bf[:]),
                             reads=[xgk, "ident_bf"], writes=["ptr2"])
                    eng = ("dve", "act")[r % 2]
                    if eng == "dve":
                        fn = lambda e, r=r, xgT_=xgT_: e.tensor_copy(xgT_[:, r * R3:(r + 1) * R3, :], ptr2[:, 0:R3, :])
                    else:
                        fn = lambda e, r=r, xgT_=xgT_: e.copy(xgT_[:, r * R3:(r + 1) * R3, :], ptr2[:, 0:R3, :])
                    P.op(eng, fn, reads=["ptr2"], writes=[xgTk + (r,)])
                for k in range(KD):
                    P.op("pe", lambda e, k=k, xgT_=xgT_, w1_=w1_: e.matmul(ph1[:, 0:DE], xgT_[:, k, :],
                                                                          w1_[:, k * DE:(k + 1) * DE],
                                                                          start=(k == 0), stop=(k == KD - 1)),
                         reads=[xgTk + (k // R3,), w1k], writes=["ph1"])
                for k in range(KD):
                    P.op("pe", lambda e, k=k, xgT_=xgT_, w3_=w3_: e.matmul(ph3[:, 0:DE], xgT_[:, k, :],
                                                                          w3_[:, k * DE:(k + 1) * DE],
                                                                          start=(k == 0), stop=(k == KD - 1)),
                         reads=[xgTk + (k // R3,), w3k], writes=["ph3"])
                s1_, s1k = s1.next()
                hd_, hdk = hdn.next()
                hT_, hTk = hdT.next()
                P.op("act", lambda e, s1_=s1_: e.activation(out=s1_[:], in_=ph1[:, 0:DE], func=AF.Silu),
                     reads=["ph1"], writes=[s1k])
                P.op("dve", lambda e, s1_=s1_, hd_=hd_: e.tensor_tensor(out=hd_[:], in0=s1_[:], in1=ph3[:, 0:DE], op=ALU.mult),
                     reads=[s1k, "ph3"], writes=[hdk])
                for kk in range(KE):
                    P.op("pe", lambda e, kk=kk, hd_=hd_: e.transpose(ptr2[:, kk, :], hd_[:, kk * 128:(kk + 1) * 128], ident_bf[:]),
                         reads=[hdk, "ident_bf"], writes=["ptr2"])
                P.op("dve", lambda e, hT_=hT_: e.tensor_copy(hT_[:, 0:KE, :], ptr2[:, 0:KE, :]),
                     reads=["ptr2"], writes=[hTk])
                ys_, ysk = ysb.next()
                for yb in range(c.NYB):
                    py_i[0] += 1
                    pyi = py_i[0] % 4
                    for kk in range(KE):
                        P.op("pe", lambda e, pyi=pyi, kk=kk, yb=yb, hT_=hT_, w2_=w2_: e.matmul(
                            py[pyi][:, 0:c.YB], hT_[:, kk, :], w2_[:, kk * D + yb * c.YB:kk * D + (yb + 1) * c.YB],
                            start=(kk == 0), stop=(kk == KE - 1)),
                            reads=[hTk, w2k], writes=[("py", pyi)])
                    eng = ("act", "dve")[yb % 2]
                    if eng == "act":
                        fn = lambda e, pyi=pyi, yb=yb, ys_=ys_: e.copy(ys_[:, yb * c.YB:(yb + 1) * c.YB], py[pyi][:, 0:c.YB])
                    else:
                        fn = lambda e, pyi=pyi, yb=yb, ys_=ys_: e.tensor_copy(ys_[:, yb * c.YB:(yb + 1) * c.YB], py[pyi][:, 0:c.YB])
                    P.op(eng, fn, reads=[("py", pyi)], writes=[ysk + (yb,)])
                P.dma("sp", ys_v[r0:r0 + 128, :], ys_[:], reads=[ysk + (yb,) for yb in range(c.NYB)],
                      writes=[("ys", ex, cbk)])

        if stop < 4:
            P.wait_all("sp")
            P.emit(es)
            return nc, P
        P.barrier()
        nc.sbuf_base, nc.sbuf_top = base_sb
        finw = sb("finw", [128, D], F32)
        h1t = Ring(nc, "h1t", 2, [128, D], F32)
        y0 = Ring(nc, "y0", 2, [128, D], F32)
        y1 = Ring(nc, "y1", 2, [128, D], F32)
        ot = Ring(nc, "ot", 2, [128, D], F32)
        ss3 = sb("ss3", [128, NT], F32)
        rs3 = sb("rs3", [128, NT], F32)
        P.dma("sp", finw[:], finw_d.ap(), writes=["finw"])
        P.op("pool", lambda e: e.memset(ss3[:], 0.0), writes=["ss3"])
        for ti in range(NT):
            h_, hk = h1t.next()
            ya, yak = y0.next()
            yb_, ybk = y1.next()
            o_, ok = ot.next()
            P.dma("sp", h_[:], h1s_d[ti * 128:(ti + 1) * 128, :], writes=[hk])
            for kx, (yt, ytk) in enumerate(((ya, yak), (yb_, ybk))):
                for gc in range(NGA):
                    P.op("pool", lambda e, yt=yt, ti=ti, kx=kx, gc=gc: e.indirect_dma_start(
                        out=yt[:, gc * GW:(gc + 1) * GW], out_offset=None, in_=ys2_d[:, :],
                        in_offset=bass.IndirectOffsetOnAxis(
                            ap=desty[:, (ti * NGA + gc) * 2 + kx:(ti * NGA + gc) * 2 + kx + 1], axis=0),
                        bounds_check=regs["ys"], oob_is_err=False),
                        reads=[], writes=[ytk + (gc,)], dma=True)
            P.op("dve", lambda e, h_=h_, ya=ya, ti=ti: e.scalar_tensor_tensor(
                out=h_[:], in0=ya[:], scalar=gate[:, 2 * ti:2 * ti + 1], in1=h_[:], op0=ALU.mult, op1=ALU.add),
                reads=[hk] + [yak + (gc,) for gc in range(D // GW)], writes=[hk])
            P.op("dve", lambda e, h_=h_, yb_=yb_, ti=ti: e.scalar_tensor_tensor(
                out=h_[:], in0=yb_[:], scalar=gate[:, 2 * ti + 1:2 * ti + 2], in1=h_[:], op0=ALU.mult, op1=ALU.add),
                reads=[hk] + [ybk + (gc,) for gc in range(D // GW)], writes=[hk])
            P.op("act", lambda e, h_=h_, o_=o_, ti=ti: e.activation(out=o_[:], in_=h_[:], func=AF.Square,
                                                                    accum_out=ss3[:, ti:ti + 1]),
                 reads=[hk, "ss3"], writes=[ok, ("ss3", ti)])
            P.op("act", lambda e, ti=ti: e.activation(out=rs3[:, ti:ti + 1], in_=ss3[:, ti:ti + 1], func=AF.Sqrt,
                                                      scale=1.0 / D, bias=epsr[:, 0:1]),
                 reads=[("ss3", ti), "epsr"], writes=[("rs3", ti)])
            P.op("dve", lambda e, ti=ti: e.reciprocal(out=rs3[:, ti:ti + 1], in_=rs3[:, ti:ti + 1]),
                 reads=[("rs3", ti)], writes=[("rs3", ti)])
            P.op("dve", lambda e, h_=h_, o_=o_, ti=ti: e.scalar_tensor_tensor(
                out=o_[:], in0=h_[:], scalar=rs3[:, ti:ti + 1], in1=finw[:], op0=ALU.mult, op1=ALU.mult),
                reads=[hk, ("rs3", ti), "finw"], writes=[ok])
            P.dma("sp", out_d[ti * 128:(ti + 1) * 128, :], o_[:], reads=[ok], writes=[("out", ti)])
        P.wait_all("sp")
        P.emit(es)
    return nc, P


def host_layout(cfg, inp):
    c = cfg
    D, KD, NJ, NE, DE, KE, NR = c.D, c.KD, c.NJ, c.NE, c.DE, c.KE, c.NR
    f = lambda a: np.ascontiguousarray(np.asarray(a, dtype=np.float32))
    w_in = f(inp["w_in"])[0]
    win = f(w_in.reshape(KD, 128, c.NEC, 128).transpose(2, 1, 0, 3)).reshape(c.NEC, 128, KD * 128)
    w_out = f(inp["w_out"])[0]
    wout = f(w_out.reshape(KD, 128, c.OC, 256).transpose(2, 1, 0, 3)).reshape(c.OC, 128, KD * 256)
    w1 = f(f(inp["w1"])[0].reshape(NE, KD, 128, DE).transpose(0, 2, 1, 3)).reshape(NE, 128, KD * DE)
    w3 = f(f(inp["w3"])[0].reshape(NE, KD, 128, DE).transpose(0, 2, 1, 3)).reshape(NE, 128, KD * DE)
    w2 = f(f(inp["w2"])[0].reshape(NE, KE, 128, D).transpose(0, 2, 1, 3)).reshape(NE, 128, KE * D)
    wrc = np.concatenate([f(inp["w_group"])[0], f(inp["w_expert_gate"])[0]], axis=1)
    wr = f(wrc.reshape(KD, 128, NR).transpose(1, 0, 2)).reshape(128, KD * NR)
    bc = lambda v: f(np.broadcast_to(f(v).reshape(1, -1), (128, f(v).size)))
    pp = np.zeros((128, c.NPP), np.float32)
    dw = f(inp["conv_dw_w"])[0]
    pp[:, c.o_cw:c.o_cw + NJ * CONV_W] = dw.reshape(CONV_W, NJ, 128).transpose(2, 1, 0).reshape(128, NJ * CONV_W)
    pp[:, c.o_cb:c.o_cb + NJ] = f(inp["conv_dw_b"])[0].reshape(NJ, 128).T
    pp[:, c.o_lnw:c.o_lnw + NJ] = f(inp["conv_ln_w"])[0].reshape(NJ, 128).T
    pp[:, c.o_lnb:c.o_lnb + NJ] = f(inp["conv_ln_b"])[0].reshape(NJ, 128).T
    lam = np.concatenate([f(inp["lam_q1"])[0], f(inp["lam_k1"])[0], f(inp["lam_q2"])[0], f(inp["lam_k2"])[0]])
    pp[:, c.o_lam:c.o_lam + 256] = lam[None, :]
    pp[:, c.o_sub] = f(inp["attn_subln_w"])[0]
    pp[:, c.o_rb:c.o_rb + NR] = np.concatenate([f(inp["b_group"])[0], f(inp["b_expert_gate"])[0].reshape(-1)])[None, :]
    return dict(win=win, wout=wout, w1=w1, w3=w3, w2=w2, wr=wr, mixw=bc(inp["mix_norm_w"]), ffnw=bc(inp["ffn_norm_w"]),
                finw=bc(inp["final_norm_w"]), pp=pp)


def run(cfg, inp, dbg=False, stop=9):
    shared = host_layout(cfg, inp)
    x = np.ascontiguousarray(np.asarray(inp["x"], dtype=np.float32))
    nc, P = build(cfg, dbg=dbg, stop=stop)
    in_maps = []
    if stop < 3:
        for k in ("w1", "w3", "w2"):
            shared.pop(k)
    for b in range(cfg.B):
        m = dict(shared)
        m["x"] = x[b]
        in_maps.append(m)
    res = run_bass_kernel_spmd(nc, in_maps, core_ids=list(range(cfg.B)))
    return res


def kernel(**inputs):
    res = run(FULL, inputs)
    return np.stack([r["out"] for r in res.results], axis=0).astype(np.float32)
```
